# Optimizing a Trainium2 kernel written in Bass

```python
import math
import jax, jax.numpy as jnp
from jax import lax
import numpy as np

D_MODEL = 2048
BATCH = 2
SEQ = 4096
DEPTH = 1
DEC_BATCH = 32
DEC_SEQ = 8
PAST_LEN = 8192
PAGE_SIZE = 128

HG_HEADS = 8
HG_KDIM = 128
HG_VDIM = 128
HG_KWIDTH = HG_HEADS * HG_KDIM
HG_WIDTH = HG_HEADS * HG_VDIM
HG_CHUNK = 64
ATT_HEAD_DIM = 128
DILATION_PAIRS = ((128, 1), (512, 4), (2048, 16))
N_GROUPS = len(DILATION_PAIRS)
ATT_KV_HEADS = 4
ATT_Q_HEADS = N_GROUPS * ATT_KV_HEADS
ATT_MAX_WINDOW = max(w for w, _ in DILATION_PAIRS)
ATT_Q_BLOCK = 128
ATT_OUT_WIDTH = ATT_KV_HEADS * ATT_HEAD_DIM
ATT_SCALE = ATT_HEAD_DIM ** -0.5
N_EXPERTS = 32
TOP_K = 4
D_FF = D_MODEL
SWIGLU_ALPHA = 1.702
SWIGLU_LIMIT = 7.0
MOE_BLOCK = 128
LN_EPS = 1e-5
RMS_EPS = 1e-5
DEEPNORM_ALPHA = (2 * DEPTH) ** 0.25
DEEPNORM_BETA = (8 * DEPTH) ** -0.25

IN_SPLITS = (
    ("hg_q", HG_KWIDTH), ("hg_f", HG_KWIDTH), ("hg_i", HG_WIDTH), ("hg_g", HG_WIDTH),
    ("att_q", ATT_Q_HEADS * ATT_HEAD_DIM), ("att_k", ATT_KV_HEADS * ATT_HEAD_DIM),
    ("att_v", ATT_KV_HEADS * ATT_HEAD_DIM), ("gate_a", D_MODEL), ("gate_b", D_MODEL),
)
IN_WIDTH = sum(w for _, w in IN_SPLITS)
SPLIT_POINTS = tuple(sum(w for _, w in IN_SPLITS[:i + 1]) for i in range(len(IN_SPLITS) - 1))
BETA_SCALED = ("hg_i", "att_v")

kernel_name = "hgrn2_dilated_attn_moe_decoder_step"


def layer_norm(x, g, b):
    xf = x.astype(jnp.float32)
    mu = jnp.mean(xf, axis=-1, keepdims=True)
    var = jnp.mean(jnp.square(xf - mu), axis=-1, keepdims=True)
    return ((xf - mu) * lax.rsqrt(var + LN_EPS) * g.astype(jnp.float32) + b.astype(jnp.float32)).astype(x.dtype)


def rms_norm(x, w):
    xf = x.astype(jnp.float32)
    return xf * lax.rsqrt(jnp.mean(jnp.square(xf), axis=-1, keepdims=True) + RMS_EPS) * w.astype(jnp.float32)


def alibi_slopes():
    n = jnp.arange(1, ATT_Q_HEADS + 1, dtype=jnp.float32)
    return jnp.power(2.0, -8.0 * n / ATT_Q_HEADS).reshape(N_GROUPS, ATT_KV_HEADS)


def hgrn2_recurrence(q, k, v, logf, s0):
    B, L, H, K = q.shape
    V = v.shape[-1]
    C = math.gcd(L, HG_CHUNK)
    n = L // C

    def chunks(a):
        return a.astype(jnp.float32).reshape(B, n, C, H, a.shape[-1]).transpose(1, 0, 3, 2, 4)

    causal = jnp.tril(jnp.ones((C, C), dtype=bool))[None, None, :, :, None]

    def step(S, inp):
        qc, kc, vc, gc = inp
        G = jnp.cumsum(gc, axis=2)
        o_inter = jnp.einsum('bhtk,bhkv->bhtv', qc * jnp.exp(G), S)
        diff = G[:, :, :, None, :] - G[:, :, None, :, :]
        decay = jnp.exp(jnp.where(causal, diff, -jnp.inf))
        scores = jnp.einsum('bhtk,bhsk,bhtsk->bhts', qc, kc, decay)
        o = o_inter + jnp.einsum('bhts,bhsv->bhtv', scores, vc)
        G_last = G[:, :, -1:, :]
        S = jnp.exp(G_last[:, :, 0, :])[..., None] * S + jnp.einsum(
            'bhsk,bhsv->bhkv', kc * jnp.exp(G_last - G), vc)
        return S, o

    S, o = lax.scan(step, s0.astype(jnp.float32), (chunks(q), chunks(k), chunks(v), chunks(logf)))
    o = o.transpose(1, 0, 3, 2, 4).reshape(B, L, H, V)
    return o, S


def dilated_window_attention(q, k_ext, v_ext, ctx_len):
    B, Lq = q.shape[:2]
    qb = Lq if Lq <= ATT_Q_BLOCK else math.gcd(Lq, ATT_Q_BLOCK)
    nb = Lq // qb
    slopes = alibi_slopes()
    qblocks = q.reshape(B, nb, qb, N_GROUPS, ATT_KV_HEADS, ATT_HEAD_DIM).transpose(1, 0, 2, 3, 4, 5)

    def block(args):
        b, qblk = args
        q_ext = ctx_len + b * qb + jnp.arange(qb)
        lses, outs = [], []
        for g, (window, dil) in enumerate(DILATION_PAIRS):
            dist = jnp.arange(window // dil + 1) * dil
            idx = q_ext[:, None] - dist[None, :]
            valid = idx >= 0
            idx = jnp.maximum(idx, 0)
            kg = k_ext[:, idx]
            vg = v_ext[:, idx]
            s = jnp.einsum('bqhd,bqjhd->bhqj', qblk[:, :, g], kg).astype(jnp.float32) * ATT_SCALE
            s = s - slopes[g][None, :, None, None] * dist.astype(jnp.float32)
            s = jnp.where(valid[None, None], s, -jnp.inf)
            lse = jax.nn.logsumexp(s, axis=-1)
            p = jnp.exp(s - lse[..., None]).astype(vg.dtype)
            outs.append(jnp.einsum('bhqj,bqjhd->bqhd', p, vg))
            lses.append(lse)
        mix = jax.nn.softmax(jnp.stack(lses), axis=0).astype(q.dtype)
        return jnp.einsum('gbhq,gbqhd->bqhd', mix, jnp.stack(outs))

    o = lax.map(block, (jnp.arange(nb), qblocks))
    return o.transpose(1, 0, 2, 3, 4).reshape(B, Lq, ATT_KV_HEADS, ATT_HEAD_DIM)


def clamped_swiglu(h):
    x_glu = jnp.minimum(h[..., ::2], SWIGLU_LIMIT)
    x_lin = jnp.clip(h[..., 1::2], -SWIGLU_LIMIT, SWIGLU_LIMIT)
    return x_glu * jax.nn.sigmoid(SWIGLU_ALPHA * x_glu) * (x_lin + 1.0)


def moe_ffn(x2d, w_router, b_router, w_gate_up, b_gate_up, w_down, b_down):
    T, D = x2d.shape
    logits = jnp.dot(x2d, w_router).astype(jnp.float32) + b_router.astype(jnp.float32)
    top_v, top_e = lax.top_k(logits, TOP_K)
    gate = jax.nn.softmax(top_v, axis=-1)
    n_assign = T * TOP_K
    e_flat = top_e.reshape(-1)
    order = jnp.argsort(e_flat)
    e_sorted = e_flat[order]
    tok_sorted = (order // TOP_K).astype(jnp.int32)
    gate_sorted = gate.reshape(-1)[order]
    counts = jnp.bincount(e_flat, length=N_EXPERTS)
    start = jnp.cumsum(counts) - counts
    padded = (counts + MOE_BLOCK - 1) // MOE_BLOCK * MOE_BLOCK
    pend = jnp.cumsum(padded)
    pstart = pend - padded
    slot = pstart[e_sorted] + jnp.arange(n_assign) - start[e_sorted]
    n_blocks = -(-n_assign // MOE_BLOCK) + N_EXPERTS
    P = n_blocks * MOE_BLOCK
    tok_pad = jnp.zeros((P,), jnp.int32).at[slot].set(tok_sorted)
    gate_pad = jnp.zeros((P,), jnp.float32).at[slot].set(gate_sorted)
    blk_expert = jnp.minimum(
        jnp.searchsorted(pend, jnp.arange(n_blocks) * MOE_BLOCK, side='right'), N_EXPERTS - 1)

    def expert_block(args):
        tok, g, e = args
        h = jnp.dot(x2d[tok], w_gate_up[e]) + b_gate_up[e]
        y = jnp.dot(clamped_swiglu(h), w_down[e]) + b_down[e]
        return y.astype(jnp.float32) * g[:, None]

    y = lax.map(expert_block, (tok_pad.reshape(n_blocks, MOE_BLOCK),
                               gate_pad.reshape(n_blocks, MOE_BLOCK), blk_expert))
    out = jax.ops.segment_sum(y.reshape(P, D), tok_pad, num_segments=T)
    return out.astype(x2d.dtype)


def decoder_layer(x, k_ctx, v_ctx, s0, lb, w_in, hg_norm_w, w_branch_a, w_branch_b, w_out,
                  ln1_g, ln1_b, w_router, b_router, w_gate_up, b_gate_up, w_down, b_down,
                  ln2_g, ln2_b):
    B, L, _ = x.shape
    hq, hf, hi, hg, aq, ak, av, ga, gb = jnp.split(jnp.dot(x, w_in), list(SPLIT_POINTS), axis=-1)
    q = jax.nn.silu(hq).reshape(B, L, HG_HEADS, HG_KDIM)
    f = lb + (1.0 - lb) * jax.nn.sigmoid(hf.astype(jnp.float32).reshape(B, L, HG_HEADS, HG_KDIM))
    o_a, s_new = hgrn2_recurrence(q, 1.0 - f, hi.reshape(B, L, HG_HEADS, HG_VDIM), jnp.log(f), s0)
    o_a = (rms_norm(o_a, hg_norm_w) * jax.nn.silu(hg.reshape(B, L, HG_HEADS, HG_VDIM).astype(jnp.float32)))
    o_a = o_a.astype(x.dtype).reshape(B, L, HG_WIDTH)
    k_b = ak.reshape(B, L, ATT_KV_HEADS, ATT_HEAD_DIM)
    v_b = av.reshape(B, L, ATT_KV_HEADS, ATT_HEAD_DIM)
    k_ext = jnp.concatenate([k_ctx.astype(k_b.dtype), k_b], axis=1)
    v_ext = jnp.concatenate([v_ctx.astype(v_b.dtype), v_b], axis=1)
    o_b = dilated_window_attention(aq.reshape(B, L, ATT_Q_HEADS, ATT_HEAD_DIM), k_ext, v_ext,
                                   k_ctx.shape[1]).reshape(B, L, ATT_OUT_WIDTH)
    merged = jax.nn.sigmoid(ga) * jnp.dot(o_a, w_branch_a) + jax.nn.sigmoid(gb) * jnp.dot(o_b, w_branch_b)
    h = layer_norm(DEEPNORM_ALPHA * x + jnp.dot(merged, w_out), ln1_g, ln1_b)
    ffn = moe_ffn(h.reshape(B * L, D_MODEL), w_router, b_router, w_gate_up, b_gate_up,
                  w_down, b_down).reshape(B, L, D_MODEL)
    y = layer_norm(DEEPNORM_ALPHA * h + ffn, ln2_g, ln2_b)
    return y, k_b, v_b, s_new


def setup_inputs(seed: int = 0) -> dict:
    key = jax.random.key(seed)
    ks = jax.random.split(key, 21)
    f32 = jnp.float32

    def nrm(k, shape, scale=1.0):
        return jax.random.normal(k, shape, f32) * scale

    att_cache = min(ATT_MAX_WINDOW, PAST_LEN)
    col_scale = jnp.concatenate([
        jnp.full((w,), DEEPNORM_BETA if name in BETA_SCALED else 1.0, f32) for name, w in IN_SPLITS])
    return {
        "x_prompt": nrm(ks[0], (BATCH, SEQ, D_MODEL)),
        "x_sample": nrm(ks[1], (DEC_BATCH, DEC_SEQ, D_MODEL)),
        "cache_attn_k": nrm(ks[2], (DEPTH, DEC_BATCH, att_cache, ATT_KV_HEADS, ATT_HEAD_DIM)),
        "cache_attn_v": nrm(ks[3], (DEPTH, DEC_BATCH, att_cache, ATT_KV_HEADS, ATT_HEAD_DIM)),
        "state_hgrn": nrm(ks[4], (DEPTH, DEC_BATCH, HG_HEADS, HG_KDIM, HG_VDIM), 0.5),
        "w_in": nrm(ks[5], (DEPTH, D_MODEL, IN_WIDTH), D_MODEL ** -0.5) * col_scale,
        "hgrn_lb_logits": nrm(ks[6], (DEPTH + 1, HG_KWIDTH), 0.5),
        "hgrn_norm_w": 1.0 + nrm(ks[7], (DEPTH, HG_VDIM), 0.02),
        "w_branch_a": nrm(ks[8], (DEPTH, HG_WIDTH, D_MODEL), HG_WIDTH ** -0.5 * DEEPNORM_BETA),
        "w_branch_b": nrm(ks[9], (DEPTH, ATT_OUT_WIDTH, D_MODEL), ATT_OUT_WIDTH ** -0.5 * DEEPNORM_BETA),
        "w_out": nrm(ks[10], (DEPTH, D_MODEL, D_MODEL), D_MODEL ** -0.5 * DEEPNORM_BETA),
        "ln1_g": 1.0 + nrm(ks[11], (DEPTH, D_MODEL), 0.02),
        "ln1_b": nrm(ks[12], (DEPTH, D_MODEL), 0.02),
        "w_router": nrm(ks[13], (DEPTH, D_MODEL, N_EXPERTS), D_MODEL ** -0.5),
        "b_router": nrm(ks[14], (DEPTH, N_EXPERTS), 0.01),
        "w_gate_up": nrm(ks[15], (DEPTH, N_EXPERTS, D_MODEL, 2 * D_FF), D_MODEL ** -0.5 * DEEPNORM_BETA),
        "b_gate_up": nrm(ks[16], (DEPTH, N_EXPERTS, 2 * D_FF), 0.01),
        "w_down": nrm(ks[17], (DEPTH, N_EXPERTS, D_FF, D_MODEL), D_FF ** -0.5 * DEEPNORM_BETA),
        "b_down": nrm(ks[18], (DEPTH, N_EXPERTS, D_MODEL), 0.01),
        "ln2_g": 1.0 + nrm(ks[19], (DEPTH, D_MODEL), 0.02),
        "ln2_b": nrm(ks[20], (DEPTH, D_MODEL), 0.02),
    }


def reference(x_prompt, x_sample, cache_attn_k, cache_attn_v, state_hgrn, w_in, hgrn_lb_logits,
              hgrn_norm_w, w_branch_a, w_branch_b, w_out, ln1_g, ln1_b, w_router, b_router,
              w_gate_up, b_gate_up, w_down, b_down, ln2_g, ln2_b):
    lower_bounds = jnp.cumsum(jax.nn.softmax(hgrn_lb_logits.astype(jnp.float32), axis=0), axis=0)
    Bp = x_prompt.shape[0]
    keep = min(ATT_MAX_WINDOW, x_prompt.shape[1])
    empty_kv = jnp.zeros((Bp, 0, ATT_KV_HEADS, ATT_HEAD_DIM), x_prompt.dtype)
    zero_state = jnp.zeros((Bp, HG_HEADS, HG_KDIM, HG_VDIM), jnp.float32)
    hp, hs = x_prompt, x_sample
    pk, pv, ps, sk, sv, ss = [], [], [], [], [], []
    for l in range(DEPTH):
        lb = lower_bounds[l].reshape(HG_HEADS, HG_KDIM)
        params = (w_in[l], hgrn_norm_w[l], w_branch_a[l], w_branch_b[l], w_out[l], ln1_g[l], ln1_b[l],
                  w_router[l], b_router[l], w_gate_up[l], b_gate_up[l], w_down[l], b_down[l],
                  ln2_g[l], ln2_b[l])
        hp, kp_l, vp_l, sp_l = decoder_layer(hp, empty_kv, empty_kv, zero_state, lb, *params)
        hs, ks_l, vs_l, ss_l = decoder_layer(hs, cache_attn_k[l], cache_attn_v[l], state_hgrn[l], lb, *params)
        pk.append(kp_l[:, -keep:])
        pv.append(vp_l[:, -keep:])
        ps.append(sp_l)
        sk.append(ks_l)
        sv.append(vs_l)
        ss.append(ss_l)
    return (hp, hs, jnp.stack(pk), jnp.stack(pv), jnp.stack(ps), jnp.stack(sk), jnp.stack(sv), jnp.stack(ss))
```

```python
import numpy as np
from contextlib import ExitStack
import concourse.bass as bass
import concourse.mybir as mybir
from concourse.alu_op_type import AluOpType as ALU
from concourse.bass_utils import run_bass_kernel_spmd

F32 = mybir.dt.float32
BF16 = mybir.dt.bfloat16
AF = mybir.ActivationFunctionType
AX = mybir.AxisListType

NCORES = 8
D = 2048
TOK = 1152
NPR = 1024
NPREF = 3072
ATT_SCALE = 128 ** -0.5
WINS = (128, 512, 2048)
DILS = (1, 4, 16)
GOFF = (0, 256, 896)
NEXP = 32
CAP = 256
ALPHA = 2 ** 0.25
BIG = 1.0e9


import os as _os
SELF_SYNC = _os.environ.get("K_SELF_SYNC", "1") == "1"


class Tl:
    __slots__ = ("name", "lw", "rd", "sem", "cnt")

    def __init__(self, name):
        self.name = name
        self.lw = []
        self.rd = []
        self.sem = None
        self.cnt = 0


class Sched:
    ENG = ("pe", "act", "dve", "pool", "sp")

    def __init__(self, nc, stack):
        self.nc = nc
        self.stack = stack
        self.ops = []
        self.eng_ops = {e: [] for e in self.ENG}
        self.bar = {e: set() for e in self.ENG}
        self.since_bar = []
        self.nsem = 0

    def T(self, name):
        return Tl(name)

    def _add(self, eng, fn, r, w, dma, semtile):
        idx = len(self.ops)
        deps = set()
        for t in r:
            deps.update(t.lw)
            if t.name.startswith("ps"):
                deps.update(i for i in t.rd if self.ops[i]["eng"] != eng)
        for t in w:
            wdma = dma and t.lw and all(self.ops[i]["dma"] for i in t.lw) and not t.rd
            if not wdma:
                deps.update(t.lw)
            deps.update(t.rd)
        deps.update(self.bar[eng])
        self.bar[eng] = set()
        deps.discard(idx)
        self.ops.append(dict(eng=eng, fn=fn, deps=deps, dma=dma, semtile=semtile, need=False, val=0))
        self.eng_ops[eng].append(idx)
        self.since_bar.append(idx)
        for t in r:
            if not dma:
                t.rd = [i for i in t.rd if self.ops[i]["dma"] or self.ops[i]["eng"] != eng]
            t.rd.append(idx)
        for t in w:
            wdma = dma and t.lw and all(self.ops[i]["dma"] for i in t.lw) and not t.rd
            if wdma:
                t.lw = t.lw + [idx]
            else:
                t.lw = [idx]
            t.rd = []
        return idx

    def op(self, eng, fn, r=(), w=()):
        return self._add(eng, fn, list(r), list(w), False, None)

    def dma(self, eng, out, in_, r=(), w=(), semtile=None):
        st = semtile if semtile is not None else (list(w) + list(r))[0]
        return self._add(eng, lambda e: e.dma_start(out=out, in_=in_), list(r), list(w), True, st)

    def barrier(self):
        last = set()
        for e in self.ENG:
            if self.eng_ops[e]:
                last.add(self.eng_ops[e][-1])
        for i in self.since_bar:
            if self.ops[i]["dma"]:
                last.add(i)
        self.since_bar = []
        for e in self.ENG:
            self.bar[e] = set(last) | self.bar[e]

    def emit(self, final_wait_ops):
        nc = self.nc
        ops = self.ops
        self.ops.append(dict(eng="sp", fn=None, deps=set(final_wait_ops), dma=False, semtile=None, need=False, val=0))
        self.eng_ops["sp"].append(len(self.ops) - 1)
        for o in ops:
            for d in o["deps"]:
                od = ops[d]
                if od["dma"] or od["eng"] != o["eng"] or (o["eng"] != "pe" and SELF_SYNC):
                    od["need"] = True
        esem = {e: self.stack.enter_context(nc.semaphore("es_" + e)) for e in self.ENG}
        ecnt = {e: 0 for e in self.ENG}
        for o in ops:
            if o["dma"]:
                t = o["semtile"]
                if t.sem is None:
                    t.sem = {}
                    t.cnt = {}
                if o["eng"] not in t.sem:
                    t.sem[o["eng"]] = self.stack.enter_context(nc.semaphore("ts%d" % self.nsem))
                    t.cnt[o["eng"]] = 0
                    self.nsem += 1
                t.cnt[o["eng"]] += 16
                o["val"] = t.cnt[o["eng"]]
                o["sem"] = t.sem[o["eng"]]
            elif o["need"]:
                ecnt[o["eng"]] += 1
                o["val"] = ecnt[o["eng"]]
                o["sem"] = esem[o["eng"]]
        self.stats = dict(ecnt=dict(ecnt), nops={e: len(v) for e, v in self.eng_ops.items()}, nsem=self.nsem)
        block = self.stack.enter_context(nc.Block())

        def run(eng_name):
            def body(e):
                waited = {}
                for idx in self.eng_ops[eng_name]:
                    o = ops[idx]
                    need_w = {}
                    for d in o["deps"]:
                        od = ops[d]
                        if not od["dma"] and od["eng"] == eng_name and (eng_name == "pe" or not SELF_SYNC):
                            continue
                        key = id(od["sem"])
                        if key not in need_w or need_w[key][1] < od["val"]:
                            need_w[key] = (od["sem"], od["val"])
                    for key, (sm, val) in need_w.items():
                        if waited.get(key, 0) >= val:
                            continue
                        waited[key] = val
                        e.wait_ge(sm, val)
                    if o["fn"] is None:
                        continue
                    inst = o["fn"](e)
                    if o["dma"]:
                        inst.then_inc(o["sem"], 16)
                    elif o["need"]:
                        inst.then_inc(o["sem"], 1)
            return body

        block.tensor(run("pe"))
        block.scalar(run("act"))
        block.vector(run("dve"))
        block.gpsimd(run("pool"))
        block.sync(run("sp"))


def build_nc(dbg=False, a2mode=9, skip_a1=False, stop_after_a2=False, a2sub=9):
    nc = bass.Bass("TRN2", target_bir_lowering=False)
    stack = ExitStack()
    S = Sched(nc, stack)

    def din(name, shape, dt=F32):
        return nc.dram_tensor(name, list(shape), dt, kind="ExternalInput").ap()

    def dout(name, shape, dt=F32):
        return nc.dram_tensor(name, list(shape), dt, kind="ExternalOutput").ap()

    xoT = din("xoT", [D, TOK])
    xo = din("xo", [TOK, D])
    xpT = din("xpT", [D, NPREF])
    kbias_d = din("kbias", [1, 2048])
    w_hg = din("w_hg", [8, D, 512])
    w_at = din("w_at", [4, D, 640])
    w_gate = din("w_gate", [D, 4096])
    w_ba = din("w_ba", [1024, D])
    w_bb = din("w_bb", [512, D])
    w_out = din("w_out", [D, D])
    w_rt = din("w_rt", [D, 32])
    b_rt = din("b_rt", [1, 32])
    w_gu = din("w_gu", [NEXP, 16, D, 256])
    b_gu = din("b_gu", [128, NEXP * 32])
    w_dn = din("w_dn", [NEXP, D, D])
    b_dn = din("b_dn", [NEXP, D])
    lnp = din("lnp", [4, D])
    lbl = din("lbl", [128, 16])
    nrmw = din("nrmw", [128, 1])
    kc_d = din("kc", [4, 2048, 512])
    vc_d = din("vc", [4, 2048, 512])
    st0_d = din("st0", [4, 8, 128, 128])
    c_idb = din("c_idb", [128, 128])
    c_idf = din("c_idf", [128, 128])
    c_m64 = din("c_m64", [128, 64])
    c_dm = din("c_dm", [128, 3072])
    c_iota = din("c_iota", [128, CAP])
    c_ustr = din("c_ustr", [128, 128])
    c_tval = din("c_tval", [128, 9])

    y_d = dout("y", [TOK, D])
    knT_d = dout("knT", [512, TOK])
    vn_d = dout("vn", [TOK, 512])
    stp_d = dout("stp", [8, 128, 128])
    sts_d = dout("sts", [4, 8, 128, 128])

    def sb(name, shape, dt):
        return stack.enter_context(nc.sbuf_tensor(name, list(shape), dt))

    ARENA_B = 180 * 1024
    arena = sb("arena", [128, ARENA_B // 2], BF16)

    def carve(off, shape, dt):
        n = int(np.prod(shape))
        esz = 4 if dt == F32 else 2
        assert off % 4 == 0 and off + n * esz <= ARENA_B, (off, shape)
        ap = arena[:, off // 2: off // 2 + n * esz // 2]
        if dt == F32:
            ap = ap.bitcast(F32)
        if len(shape) == 2:
            ap = ap.rearrange("p (a b) -> p a b", b=shape[1])
        elif len(shape) == 3:
            ap = ap.rearrange("p (a b c) -> p a b c", b=shape[1], c=shape[2])
        return ap

    psb = [stack.enter_context(nc.psum_tensor("ps%d" % i, [128, 512], F32)) for i in range(8)]
    PS = [S.T("ps%d" % i) for i in range(8)]

    idb = sb("idb", [128, 128], BF16); T_c = S.T("consts")
    idf = sb("idf", [128, 128], F32)
    m64 = sb("m64", [128, 64], F32)
    dm = sb("dm", [128, 3072], F32)
    iota = sb("iota", [128, CAP], F32)
    ustr = sb("ustr", [128, 128], BF16)
    onesb = sb("onesb", [128, 128], BF16)
    onesf = sb("onesf", [128, 512], F32)
    tval = sb("tval", [128, 9], F32)
    lbt = sb("lbt", [128, 16], F32)
    lbv = sb("lbv", [128, 8], F32)
    oml = sb("oml", [128, 8], F32)
    nrm = sb("nrm", [128, 1], F32)
    kbias = sb("kbias_sb", [1, 2048], BF16)
    bgu = sb("bgu", [128, NEXP * 32], F32)

    outs_final = []

    def cload(dst, src, eng="sp"):
        S.dma(eng, dst, src, w=[T_c])

    cload(idb[:], c_idb, "pool"); cload(idf[:], c_idf); cload(m64[:], c_m64); cload(dm[:], c_dm)
    cload(iota[:], c_iota); cload(ustr[:], c_ustr, "pool"); cload(tval[:], c_tval)
    cload(lbt[:], lbl); cload(nrm[:], nrmw); cload(kbias[:], kbias_d, "pool"); cload(bgu[:], b_gu)
    S.op("dve", lambda e: e.memset(onesb[:], 1.0), w=[T_c])
    S.op("dve", lambda e: e.memset(onesf[:], 1.0), w=[T_c])
    S.op("dve", lambda e: e.tensor_tensor(out=lbv[:], in0=lbt[:, 0:8], in1=lbt[:, 8:16], op=ALU.subtract), r=[T_c], w=[T_c])
    S.op("act", lambda e: e.activation(out=lbv[:], in_=lbv[:], func=AF.Sigmoid), r=[T_c], w=[T_c])
    S.op("dve", lambda e: e.tensor_scalar(out=oml[:], in0=lbv[:], scalar1=-1.0, scalar2=1.0, op0=ALU.mult, op1=ALU.add), r=[T_c], w=[T_c])

    O_XOT = 0
    O_OAT = 36864
    O_OBT = O_OAT + 18432
    O_XP = O_OBT + 9216
    O_W = O_XP + 32768
    O_TMP = O_W + 40960
    xoT_sb = carve(O_XOT, [16, TOK], BF16); T_xoT = S.T("xoT")
    oaT = carve(O_OAT, [8, TOK], BF16); T_oaT = S.T("oaT")
    obT = carve(O_OBT, [4, TOK], BF16); T_obT = S.T("obT")
    xp_sb = [carve(O_XP + i * 16384, [16, 512], BF16) for i in range(2)]; T_xp = [S.T("xp0"), S.T("xp1")]
    w_sb = [carve(O_W + i * 20480, [16, 640], BF16) for i in range(2)]; T_w = [S.T("w0"), S.T("w1")]

    S.op("dve", lambda e: e.memset(oaT[:, :, 1056:TOK], 0.0), w=[T_oaT])
    S.op("dve", lambda e: e.memset(obT[:, :, 1056:TOK], 0.0), w=[T_obT])
    xoT_v = xoT.rearrange("(k p) t -> p k t", p=128)
    xpT_v = xpT.rearrange("(k p) t -> p k t", p=128)
    for k0 in range(0, 16, 4):
        S.dma("pool", xoT_sb[:, k0:k0 + 4, :], xoT_v[:, k0:k0 + 4, :], w=[T_xoT])

    o = O_TMP
    def tf(n=512):
        nonlocal o
        a = carve(o, [n], F32); o += n * 4
        return a
    def tb(n=512):
        nonlocal o
        a = carve(o, [n], BF16); o += n * 2
        return a
    t_sig = tf(); t_q = tf(); t_f = tf(); t_lf = tf(); t_kk = tf(); t_G = tf(); t_e = tf()
    t_gs = tb(); t_vT = tb(); t_qt = tb(); t_kt = tb(); t_khT = tb()
    t_dec = tf(8)
    kh64 = carve(o, [128], BF16); o += 256
    v64 = carve(o, [128], BF16); o += 256
    sc64 = carve(o, [64], BF16); o += 128
    kh128 = carve(o, [128], BF16); o += 256
    v128 = carve(o, [128], BF16); o += 256
    kh8 = carve(o, [128], BF16); o += 256
    v8 = carve(o, [128], BF16); o += 256
    sc8 = carve(o, [8], BF16); o += 16
    on_sb = carve(o, [128], BF16); o += 256
    ssq = tf(2); rstd = tf(2); sqj = tf(128)
    Sst = [carve(o + i * 512, [128], F32) for i in range(5)]; o += 5 * 512
    Sbf = [carve(o + i * 256, [128], BF16) for i in range(5)]; o += 5 * 256
    T_h = S.T("hg_tmp")
    T_c64 = S.T("hg_chunk")
    T_S = [S.T("S%d" % i) for i in range(5)]
    for t_ in (kh64, v64, sc64, kh8, v8, sc8):
        S.op("dve", (lambda t_: lambda e: e.memset(t_, 0.0))(t_), w=[T_c64])

    def load_w(buf, src_ap, ncols):
        S.dma("pool", w_sb[buf][:, :, 0:ncols], src_ap.rearrange("(k p) c -> p k c", p=128), w=[T_w[buf]])

    def hgrn_group(h, wbuf, src_sb, T_src, c0, N, C, own, Si, out_col0):
        W = w_sb[wbuf]
        nb = N // C
        blocks = (0, 1, 2, 3) if own else (1, 2)
        for bi, blk in enumerate(blocks):
            for k in range(16):
                S.op("pe", (lambda blk, k, bi: lambda e: e.matmul(psb[bi][:, 0:N], lhsT=W[:, k, blk * 128:(blk + 1) * 128],
                                                               rhs=src_sb[:, k, c0:c0 + N], start=(k == 0), stop=(k == 15)))(blk, k, bi),
                     r=[T_w[wbuf], T_src], w=[PS[bi]])
        yield 1
        if own:
            PQ, PF, PI, PG = psb[0], psb[1], psb[2], psb[3]; TQ, TF, TI, TG = PS[0], PS[1], PS[2], PS[3]
        else:
            PF, PI = psb[0], psb[1]; TF, TI = PS[0], PS[1]
        sl = slice(0, N)
        if own:
            S.op("act", lambda e: e.activation(out=t_sig[:, sl], in_=PQ[:, sl], func=AF.Sigmoid), r=[TQ], w=[T_h])
            S.op("dve", lambda e: e.tensor_tensor(out=t_q[:, sl], in0=PQ[:, sl], in1=t_sig[:, sl], op=ALU.mult), r=[TQ, T_h], w=[T_h])
            S.op("act", lambda e: e.activation(out=t_sig[:, sl], in_=PG[:, sl], func=AF.Sigmoid), r=[TG, T_h], w=[T_h])
            S.op("dve", lambda e: e.tensor_tensor(out=t_gs[:, sl], in0=PG[:, sl], in1=t_sig[:, sl], op=ALU.mult), r=[TG, T_h], w=[T_h])
        S.op("act", lambda e: e.activation(out=t_sig[:, sl], in_=PF[:, sl], func=AF.Sigmoid), r=[TF, T_h], w=[T_h])
        S.op("dve", lambda e: e.tensor_scalar(out=t_f[:, sl], in0=t_sig[:, sl], scalar1=oml[:, h:h + 1], scalar2=lbv[:, h:h + 1],
                                              op0=ALU.mult, op1=ALU.add), r=[T_h, T_c], w=[T_h])
        S.op("act", lambda e: e.copy(out=t_vT[:, sl], in_=PI[:, sl]), r=[TI, T_h], w=[T_h])
        yield 2
        S.op("act", lambda e: e.activation(out=t_lf[:, sl], in_=t_f[:, sl], func=AF.Ln), r=[T_h], w=[T_h])
        S.op("dve", lambda e: e.tensor_scalar(out=t_kk[:, sl], in0=t_f[:, sl], scalar1=-1.0, scalar2=1.0, op0=ALU.mult, op1=ALU.add), r=[T_h], w=[T_h])
        for c in range(nb):
            cs = slice(c * C, (c + 1) * C)
            S.op("dve", (lambda cs: lambda e: e.tensor_tensor_scan(out=t_G[:, cs], data0=onesf[:, cs], data1=t_lf[:, cs], initial=0.0,
                                                                   op0=ALU.mult, op1=ALU.add))(cs), r=[T_h, T_c], w=[T_h])
        if own:
            S.op("act", lambda e: e.activation(out=t_e[:, sl], in_=t_G[:, sl], func=AF.Exp), r=[T_h], w=[T_h])
            S.op("dve", lambda e: e.tensor_tensor(out=t_qt[:, sl], in0=t_q[:, sl], in1=t_e[:, sl], op=ALU.mult), r=[T_h], w=[T_h])
            S.op("act", lambda e: e.activation(out=t_e[:, sl], in_=t_G[:, sl], func=AF.Exp, scale=-1.0), r=[T_h], w=[T_h])
            S.op("dve", lambda e: e.tensor_tensor(out=t_kt[:, sl], in0=t_kk[:, sl], in1=t_e[:, sl], op=ALU.mult), r=[T_h], w=[T_h])
        for c in range(nb):
            cs = slice(c * C, (c + 1) * C)
            last = slice((c + 1) * C - 1, (c + 1) * C)
            S.op("act", (lambda cs, last: lambda e: e.activation(out=t_e[:, cs], in_=t_G[:, cs], func=AF.Exp, scale=-1.0, bias=t_G[:, last]))(cs, last),
                 r=[T_h], w=[T_h])
            S.op("act", (lambda c, last: lambda e: e.activation(out=t_dec[:, c:c + 1], in_=t_G[:, last], func=AF.Exp))(c, last), r=[T_h], w=[T_h])
        S.op("dve", lambda e: e.tensor_tensor(out=t_khT[:, sl], in0=t_kk[:, sl], in1=t_e[:, sl], op=ALU.mult), r=[T_h], w=[T_h])
        if C == 8:
            kh_c, v_c, sc_c = kh8, v8, sc8
        elif C == 128:
            kh_c, v_c, sc_c = kh128, v128, None
        else:
            kh_c, v_c, sc_c = kh64, v64, sc64
        P4b = psb[4][:].bitcast(BF16)
        P5b = psb[5][:].bitcast(BF16)
        for c in range(nb):
            si = Si[c] if isinstance(Si, (list, tuple)) else Si
            Ssb, Sb, TS = Sst[si], Sbf[si], T_S[si]
            cs = slice(c * C, (c + 1) * C)
            S.op("pe", (lambda cs: lambda e: e.transpose(out=P4b[0:C, 0:128], in_=t_khT[:, cs], identity=idb[:]))(cs), r=[T_h, T_c], w=[PS[4]])
            S.op("pe", (lambda cs: lambda e: e.transpose(out=P4b[0:C, 128:256], in_=t_vT[:, cs], identity=idb[:]))(cs), r=[T_h, T_c], w=[PS[4]])
            S.op("dve", lambda e: e.tensor_copy(out=kh_c[0:C, :], in_=P4b[0:C, 0:128]), r=[PS[4]], w=[T_c64])
            S.op("act", lambda e: e.copy(out=v_c[0:C, :], in_=P4b[0:C, 128:256]), r=[PS[4]], w=[T_c64])
            if own:
                S.op("pe", (lambda cs: lambda e: e.matmul(psb[6][0:C, 0:C], lhsT=t_kt[:, cs], rhs=t_qt[:, cs], start=True, stop=True))(cs),
                     r=[T_h], w=[PS[6]])
                S.op("dve", lambda e: e.tensor_tensor(out=sc_c[0:C, 0:C], in0=psb[6][0:C, 0:C], in1=m64[0:C, 0:C], op=ALU.mult),
                     r=[PS[6], T_c], w=[T_c64])
                S.op("pe", (lambda cs, Sb: lambda e: e.matmul(psb[7][0:C, 0:128], lhsT=t_qt[:, cs], rhs=Sb[:, :], start=True, stop=False))(cs, Sb),
                     r=[T_h, TS], w=[PS[7]])
                S.op("pe", lambda e: e.matmul(psb[7][0:C, 0:128], lhsT=sc_c[:, 0:C], rhs=v_c[:, :], start=False, stop=True),
                     r=[T_c64], w=[PS[7]])
                S.op("act", lambda e: e.activation(out=sqj[0:C, 0:128], in_=psb[7][0:C, 0:128], func=AF.Square, accum_out=ssq[0:C, 0:1]),
                     r=[PS[7]], w=[T_c64])
                S.op("dve", lambda e: e.tensor_scalar(out=rstd[0:C, 0:1], in0=ssq[0:C, 0:1], scalar1=1.0 / 128, scalar2=1e-5, op0=ALU.mult, op1=ALU.add),
                     r=[T_c64], w=[T_c64])
                S.op("act", lambda e: e.activation(out=rstd[0:C, 0:1], in_=rstd[0:C, 0:1], func=AF.Sqrt), r=[T_c64], w=[T_c64])
                S.op("dve", lambda e: e.reciprocal(out=rstd[0:C, 0:1], in_=rstd[0:C, 0:1]), r=[T_c64], w=[T_c64])
                S.op("dve", lambda e: e.tensor_scalar(out=on_sb[0:C, :], in0=psb[7][0:C, 0:128], scalar1=rstd[0:C, 0:1], scalar2=1.0, op0=ALU.mult, op1=ALU.mult),
                     r=[PS[7], T_c64], w=[T_c64])
                S.op("pe", lambda e: e.transpose(out=P5b[:, 0:C], in_=on_sb[0:C, :], identity=idb[0:C, 0:C]), r=[T_c64, T_c], w=[PS[5]])
                oc = slice(out_col0 + c * C, out_col0 + (c + 1) * C)
                S.op("dve", (lambda cs, oc: lambda e: e.scalar_tensor_tensor(out=oaT[:, h, oc], in0=P5b[:, 0:C], scalar=nrm[:, 0:1], in1=t_gs[:, cs],
                                                                            op0=ALU.mult, op1=ALU.mult))(cs, oc), r=[PS[5], T_h, T_c], w=[T_oaT])
            S.op("pe", lambda e: e.matmul(psb[6][:, 128:256], lhsT=kh_c[:, :], rhs=v_c[:, :], start=True, stop=True), r=[T_c64], w=[PS[6]])
            S.op("dve", (lambda c, Ssb: lambda e: e.scalar_tensor_tensor(out=Ssb[:, :], in0=Ssb[:, :], scalar=t_dec[:, c:c + 1], in1=psb[6][:, 128:256],
                                                                         op0=ALU.mult, op1=ALU.add))(c, Ssb), r=[PS[6], T_h, TS], w=[TS])
            S.op("act", (lambda Ssb, Sb: lambda e: e.copy(out=Sb[:, :], in_=Ssb[:, :]))(Ssb, Sb), r=[TS], w=[TS])

    xp_i = 0
    tasks = []
    for h in range(0 if skip_a1 else 8):
        wb = h % 2
        for g in range(6):
            xb = xp_i % 2; xp_i += 1
            def pre(h=h, wb=wb, g=g, xb=xb):
                if g == 0:
                    load_w(wb, w_hg[h], 512)
                    S.op("dve", lambda e: e.memset(Sst[0][:, :], 0.0), w=[T_S[0]])
                    S.op("dve", lambda e: e.memset(Sbf[0][:, :], 0.0), w=[T_S[0]])
                S.dma("pool", xp_sb[xb][:, :, :], xpT_v[:, :, g * 512:(g + 1) * 512], w=[T_xp[xb]])
            tasks.append((pre, hgrn_group(h, wb, xp_sb[xb], T_xp[xb], 0, 512, 128, False, 0, 0), None))
        for g in range(2):
            def post(h=h, g=g):
                if g == 1:
                    outs_final.append(S.dma("sp", stp_d[h], Sst[0][:, :], r=[T_S[0]]))
            tasks.append((None, hgrn_group(h, wb, xoT_sb, T_xoT, g * 512, 512, 64, True, 0, g * 512), post))
        def pre(h=h):
            for j in range(4):
                S.dma("sp", Sst[1 + j][:, :], st0_d[j, h], w=[T_S[1 + j]])
                S.op("act", (lambda j: lambda e: e.copy(out=Sbf[1 + j][:, :], in_=Sst[1 + j][:, :]))(j), r=[T_S[1 + j]], w=[T_S[1 + j]])
        def post(h=h):
            for j in range(4):
                outs_final.append(S.dma("sp", sts_d[j, h], Sst[1 + j][:, :], r=[T_S[1 + j]]))
        tasks.append((pre, hgrn_group(h, wb, xoT_sb, T_xoT, 1024, 32, 8, True, [1, 2, 3, 4], 1024), post))

    def finish(t):
        for _ in t[1]:
            pass
        if t[2] is not None:
            t[2]()
    prev = None
    for t in tasks:
        if t[0] is not None:
            t[0]()
        next(t[1])
        if prev is not None:
            finish(prev)
        next(t[1])
        prev = t
    if prev is not None:
        finish(prev)


    S.barrier()
    O_XP2 = O_XP
    O_W2 = O_XP2 + 16384
    o = O_W2 + 20480
    xp2 = [carve(O_XP2 + i * 8192, [16, 256], BF16) for i in range(2)]; T_xp2 = [S.T("xp2a"), S.T("xp2b")]
    w2 = carve(O_W2, [16, 640], BF16); T_w2 = S.T("w2")
    def cv(shape, dt):
        nonlocal o
        a = carve(o, shape, dt); o += int(np.prod(shape)) * (4 if dt == F32 else 2)
        return a
    QT = cv([3, TOK], BF16); T_QT = S.T("QT")
    KT = cv([3072], BF16); T_KT = S.T("KT")
    KTs = cv([2176], BF16); T_KTs = S.T("KTs")
    Vt = cv([24, 128], BF16); T_V = S.T("V")
    Vs = cv([17, 128], BF16); T_Vs = S.T("Vs")
    Kc = cv([16, 128], BF16); T_Kc = S.T("Kc")
    zt = cv([3072], F32); T_z = S.T("z")
    Pt = cv([3072], BF16); T_P = S.T("P")
    PTs = cv([24, 128], BF16); T_PT = S.T("PT")
    kst = cv([512], F32); T_kst = S.T("kst")
    vst = cv([128], F32); T_vst = S.T("vst")
    ksam = cv([32], BF16); T_ksam = S.T("ksam")
    ob = cv([128], BF16); mx = cv([2], F32); negm = cv([2], F32); rsum = cv([2], F32); rinv = cv([2], F32); T_att = S.T("att_small")
    kbB = cv([2048], F32); T_kbB = S.T("kbB")
    S.dma("sp", kbB[:, :], kbias_d[0:1, :].broadcast_to([128, 2048]), w=[T_kbB])
    S.op("dve", lambda e: e.memset(KTs[:, :], 0.0), w=[T_KTs])
    S.op("dve", lambda e: e.memset(Vs[:, 16, :], 0.0), w=[T_Vs])
    evac_rr = [0]
    def evac(out, in_, r, w):
        evac_rr[0] ^= 1
        if evac_rr[0]:
            S.op("act", lambda e: e.mul(out=out, in_=in_, mul=1.0), r=r, w=w)
        else:
            S.op("dve", lambda e: e.tensor_scalar(out=out, in0=in_, scalar1=1.0, scalar2=0.0, op0=ALU.mult, op1=ALU.add), r=r, w=w)

    def attn_unit(h, M, qc, Ksrc, T_K, kbase, Vsrc, T_Vsrc, vbase, use_kb):
        rr = 0
        for g in range(3):
            Wg = WINS[g]; nk = Wg + 128; k0 = kbase + 2048 - Wg
            cgh = -(2.0 ** (-8.0 * (g * 4 + h + 1) / 12.0)) / ATT_SCALE
            for p0 in range(0, nk, 512):
                n = min(512, nk - p0)
                bank = rr % 3; rr += 1
                kc0 = k0 + p0
                nn = max(0, min(2048, kc0 + n) - kc0) if (use_kb and a2mode >= 3) else 0
                S.op("pe", (lambda g, kc0, n, bank, nn: lambda e: e.matmul(psb[bank][0:M, 0:n], lhsT=QT[:, g, qc:qc + M], rhs=Ksrc[:, kc0:kc0 + n],
                                                                        start=True, stop=True))(g, kc0, n, bank, nn), r=[T_QT, T_K], w=[PS[bank]])
                zc = GOFF[g] + p0
                S.op("dve", (lambda zc, n, bank, cgh: lambda e: e.scalar_tensor_tensor(out=zt[0:M, zc:zc + n], in0=dm[0:M, zc:zc + n], scalar=cgh,
                                                                                    in1=psb[bank][0:M, 0:n], op0=ALU.mult, op1=ALU.add))(zc, n, bank, cgh),
                     r=[PS[bank], T_c], w=[T_z])
                if nn > 0:
                    S.op("dve", (lambda zc, nn, kc0: lambda e: e.tensor_tensor(out=zt[0:M, zc:zc + nn], in0=zt[0:M, zc:zc + nn], in1=kbB[0:M, kc0:kc0 + nn], op=ALU.add))(zc, nn, kc0),
                         r=[T_z, T_kbB], w=[T_z])
        S.op("dve", lambda e: e.tensor_reduce(out=mx[0:M, 0:1], in_=zt[0:M, :], axis=AX.X, op=ALU.max), r=[T_z], w=[T_att])
        S.op("dve", lambda e: e.tensor_scalar(out=negm[0:M, 0:1], in0=mx[0:M, 0:1], scalar1=-ATT_SCALE, scalar2=0.0, op0=ALU.mult, op1=ALU.add), r=[T_att], w=[T_att])
        S.op("act", lambda e: e.activation(out=Pt[0:M, :], in_=zt[0:M, :], func=AF.Exp, bias=negm[0:M, 0:1], scale=ATT_SCALE, accum_out=rsum[0:M, 0:1]),
             r=[T_z, T_att], w=[T_P, T_att])
        S.op("dve", lambda e: e.reciprocal(out=rinv[0:M, 0:1], in_=rsum[0:M, 0:1]), r=[T_att], w=[T_att])
        for bk in range(3):
            pb = 3 + (bk % 2)
            Pv = psb[pb][:].bitcast(BF16)
            for jj in range(8):
                j = bk * 8 + jj
                S.op("pe", (lambda j, jj, Pv: lambda e: e.transpose(out=Pv[:, jj * 128:jj * 128 + M], in_=Pt[0:M, j * 128:(j + 1) * 128], identity=idb[0:M, 0:M]))(j, jj, Pv),
                     r=[T_P, T_c], w=[PS[pb]])
            if M == 128:
                evac(PTs[:, bk * 8:(bk + 1) * 8, :], Pv[:, :].rearrange("p (a b) -> p a b", b=128), [PS[pb]], [T_PT])
            else:
                evac(PTs[:, bk * 8:(bk + 1) * 8, 0:M], Pv[:, :].rearrange("p (a b) -> p a b", b=128)[:, :, 0:M], [PS[pb]], [T_PT])
        for j in range(24):
            if j < 2:
                g, loc = 0, j
            elif j < 7:
                g, loc = 1, j - 2
            else:
                g, loc = 2, j - 7
            vt = vbase + 16 - WINS[g] // 128 + loc
            S.op("pe", (lambda j, vt: lambda e: e.matmul(psb[5][0:M, 0:128], lhsT=PTs[:, j, 0:M], rhs=Vsrc[:, vt, :], start=(j == 0), stop=(j == 23)))(j, vt),
                 r=[T_PT, T_Vsrc], w=[PS[5]])
        S.op("dve", lambda e: e.tensor_scalar(out=ob[0:M, :], in0=psb[5][0:M, 0:128], scalar1=rinv[0:M, 0:1], scalar2=1.0, op0=ALU.mult, op1=ALU.mult),
             r=[PS[5], T_att], w=[T_att])
        P6 = psb[6][:].bitcast(BF16)
        S.op("pe", lambda e: e.transpose(out=P6[:, 0:M], in_=ob[0:M, :], identity=idb[0:M, 0:M]), r=[T_att, T_c], w=[PS[6]])
        S.op("act", lambda e: e.copy(out=obT[:, h, qc:qc + M], in_=P6[:, 0:M]), r=[PS[6]], w=[T_obT])

    xp_i = 0
    for h in range(4 if a2sub >= 1 else 0):
        w_at_v = w_at[h].rearrange("(k p) c -> p k c", p=128)
        S.dma("pool", w2[:, :, 0:512], w_at_v[:, :, 0:512], w=[T_w2])
        S.dma("pool", w2[:, :, 512:640], w_at_v[:, :, 512:640], w=[T_w2])
        SUB2 = int(_os.environ.get("K_SUB2", "9"))
        for (c0, N) in ((0, 512), (512, 512), (1024, 128)) if SUB2 >= 1 else ():
            for blk in range(4):
                for k in range(16):
                    S.op("pe", (lambda blk, k, c0, N: lambda e: e.matmul(psb[blk][:, 0:N], lhsT=w2[:, k, blk * 128:(blk + 1) * 128], rhs=xoT_sb[:, k, c0:c0 + N],
                                                                     start=(k == 0), stop=(k == 15)))(blk, k, c0, N), r=[T_w2, T_xoT], w=[PS[blk]])
            if SUB2 < 2:
                continue
            for g in range(3):
                evac(QT[:, g, c0:c0 + N], psb[g][:, 0:N], [PS[g]], [T_QT])
            if SUB2 < 3:
                continue
            if c0 < 1024:
                S.op("act", (lambda c0, N: lambda e: e.copy(out=KT[:, 2048 + c0:2048 + c0 + N], in_=psb[3][:, 0:N]))(c0, N), r=[PS[3]], w=[T_KT])
            else:
                S.op("act", lambda e: e.copy(out=ksam[:, 0:32], in_=psb[3][:, 0:32]), r=[PS[3]], w=[T_ksam])
            if _os.environ.get("K_SUB3", "") == "a":
                continue
            S.op("dve", (lambda N: lambda e: e.tensor_scalar(out=kst[:, 0:N], in0=psb[3][:, 0:N], scalar1=1.0, scalar2=0.0, op0=ALU.mult, op1=ALU.add))(N), r=[PS[3], T_KT, T_ksam], w=[T_kst])
            if a2sub >= 2:
                outs_final.append(S.dma("sp", knT_d[h * 128:(h + 1) * 128, c0:c0 + N], kst[:, 0:N], r=[T_kst]))
        for i in range(9 if a2sub >= 3 else 0):
            for k in range(16):
                S.op("pe", (lambda i, k: lambda e: e.matmul(psb[4][:, 0:128], lhsT=xoT_sb[:, k, i * 128:(i + 1) * 128], rhs=w2[:, k, 512:640],
                                                           start=(k == 0), stop=(k == 15)))(i, k), r=[T_w2, T_xoT], w=[PS[4]])
            if i < 8:
                S.op("act", (lambda i: lambda e: e.copy(out=Vt[:, 16 + i, :], in_=psb[4][:, 0:128]))(i), r=[PS[4]], w=[T_V])
            S.op("dve", lambda e: e.tensor_scalar(out=vst[:, :], in0=psb[4][:, 0:128], scalar1=1.0, scalar2=0.0, op0=ALU.mult, op1=ALU.add), r=[PS[4]], w=[T_vst])
            outs_final.append(S.dma("sp", vn_d[i * 128:(i + 1) * 128, h * 128:(h + 1) * 128], vst[:, :], r=[T_vst]))
        for g in range(8 if a2sub >= 4 else 0):
            xb = xp_i % 2; xp_i += 1
            S.dma("pool", xp2[xb][:, :, :], xpT_v[:, :, 1024 + g * 256:1024 + (g + 1) * 256], w=[T_xp2[xb]])
            for k in range(16):
                S.op("pe", (lambda k, xb: lambda e: e.matmul(psb[0][:, 0:256], lhsT=w2[:, k, 384:512], rhs=xp2[xb][:, k, :], start=(k == 0), stop=(k == 15)))(k, xb),
                     r=[T_w2, T_xp2[xb]], w=[PS[0]])
            S.op("act", (lambda g: lambda e: e.copy(out=KT[:, g * 256:(g + 1) * 256], in_=psb[0][:, 0:256]))(g), r=[PS[0]], w=[T_KT])
            for t in range(2):
                for k in range(16):
                    S.op("pe", (lambda k, xb, t: lambda e: e.matmul(psb[1 + t][:, 0:128], lhsT=xp2[xb][:, k, t * 128:(t + 1) * 128], rhs=w2[:, k, 512:640],
                                                                  start=(k == 0), stop=(k == 15)))(k, xb, t), r=[T_w2, T_xp2[xb]], w=[PS[1 + t]])
                S.op("dve", (lambda g, t: lambda e: e.tensor_copy(out=Vt[:, 2 * g + t, :], in_=psb[1 + t][:, 0:128]))(g, t), r=[PS[1 + t]], w=[T_V])
        for b in range(8 if a2mode >= 2 else 0):
            attn_unit(h, 128, b * 128, KT, T_KT, b * 128, Vt, T_V, b, True)
        for j in range(4 if a2mode >= 4 else 0):
            S.dma("pool", Kc[:, :, :], kc_d[j].rearrange("(t p) c -> p t c", p=128)[:, :, h * 128:(h + 1) * 128], w=[T_Kc])
            S.dma("pool", Vs[:, 0:16, :], vc_d[j].rearrange("(t p) c -> p t c", p=128)[:, :, h * 128:(h + 1) * 128], w=[T_Vs])
            for bk in range(2):
                Pv = psb[bk][:].bitcast(BF16)
                for tt in range(8):
                    t = bk * 8 + tt
                    S.op("pe", (lambda t, tt, Pv: lambda e: e.transpose(out=Pv[:, tt * 128:(tt + 1) * 128], in_=Kc[:, t, :], identity=idb[:, :]))(t, tt, Pv),
                         r=[T_Kc, T_c], w=[PS[bk]])
                evac(KTs[:, bk * 1024:(bk + 1) * 1024], Pv[:, :], [PS[bk]], [T_KTs])
            S.op("dve", (lambda j: lambda e: e.tensor_scalar(out=KTs[:, 2048:2056], in0=ksam[:, 8 * j:8 * j + 8], scalar1=1.0, scalar2=0.0, op0=ALU.mult, op1=ALU.add))(j), r=[T_ksam], w=[T_KTs])
            for k in range(16):
                S.op("pe", (lambda k, j: lambda e: e.matmul(psb[2][0:8, 0:128], lhsT=xoT_sb[:, k, 1024 + 8 * j:1032 + 8 * j], rhs=w2[:, k, 512:640],
                                                           start=(k == 0), stop=(k == 15)))(k, j), r=[T_w2, T_xoT], w=[PS[2]])
            S.op("act", lambda e: e.copy(out=Vs[0:8, 16, :], in_=psb[2][0:8, 0:128]), r=[PS[2]], w=[T_Vs])
            attn_unit(h, 8, 1024 + 8 * j, KTs, T_KTs, 0, Vs, T_Vs, 0, False)


    if stop_after_a2:
        S.barrier()
        S.emit(outs_final)
        print("[kernel] sched stats", S.stats, flush=True)
        return nc, stack
    S.barrier()
    O_WB = 64512
    O_TB = O_WB + 2 * 22528
    O_MT = 147456
    mT = carve(O_MT, [16, TOK], BF16); T_mT = S.T("mT")
    wb_sb = []
    for i in range(2):
        base = O_WB + i * 22528
        wb_sb.append((carve(base, [16, 256], BF16), carve(base + 8192, [16, 256], BF16), carve(base + 16384, [8, 256], BF16), carve(base + 20480, [4, 256], BF16)))
    T_wb = [S.T("wb0"), S.T("wb1")]
    sga = [carve(O_TB + i * 6144, [512], F32) for i in range(2)]
    sgb = [carve(O_TB + i * 6144 + 2048, [512], F32) for i in range(2)]
    tm1 = [carve(O_TB + i * 6144 + 4096, [512], F32) for i in range(2)]
    T_tb = [S.T("tb0"), S.T("tb1")]
    w_ba_v = w_ba.rearrange("(k p) c -> p k c", p=128)
    w_bb_v = w_bb.rearrange("(k p) c -> p k c", p=128)
    w_gate_v = w_gate.rearrange("(k p) c -> p k c", p=128)
    it = 0
    for gi in range(8):
        bi = gi % 2
        ga_w, gb_w, wa_w, wb_w = wb_sb[bi]
        S.dma("pool", ga_w[:, :, :], w_gate_v[:, :, gi * 256:(gi + 1) * 256], w=[T_wb[bi]])
        S.dma("pool", gb_w[:, :, :], w_gate_v[:, :, 2048 + gi * 256:2048 + (gi + 1) * 256], w=[T_wb[bi]])
        S.dma("pool", wa_w[:, :, :], w_ba_v[:, :, gi * 256:(gi + 1) * 256], w=[T_wb[bi]])
        S.dma("pool", wb_w[:, :, :], w_bb_v[:, :, gi * 256:(gi + 1) * 256], w=[T_wb[bi]])
        for cc in range(2):
            for (c0, N) in ((0, 512), (512, 512), (1024, 128)):
                pb = (it % 2) * 4; tb = it % 2; it += 1
                cs_ = slice(cc * 128, (cc + 1) * 128)
                for k in range(16):
                    S.op("pe", (lambda k, pb, cs_, c0, N, ga_w: lambda e: e.matmul(psb[pb][:, 0:N], lhsT=ga_w[:, k, cs_], rhs=xoT_sb[:, k, c0:c0 + N], start=(k == 0), stop=(k == 15)))(k, pb, cs_, c0, N, ga_w),
                         r=[T_wb[bi], T_xoT], w=[PS[pb]])
                for k in range(16):
                    S.op("pe", (lambda k, pb, cs_, c0, N, gb_w: lambda e: e.matmul(psb[pb + 1][:, 0:N], lhsT=gb_w[:, k, cs_], rhs=xoT_sb[:, k, c0:c0 + N], start=(k == 0), stop=(k == 15)))(k, pb, cs_, c0, N, gb_w),
                         r=[T_wb[bi], T_xoT], w=[PS[pb + 1]])
                for k in range(8):
                    S.op("pe", (lambda k, pb, cs_, c0, N, wa_w: lambda e: e.matmul(psb[pb + 2][:, 0:N], lhsT=wa_w[:, k, cs_], rhs=oaT[:, k, c0:c0 + N], start=(k == 0), stop=(k == 7)))(k, pb, cs_, c0, N, wa_w),
                         r=[T_wb[bi], T_oaT], w=[PS[pb + 2]])
                for k in range(4):
                    S.op("pe", (lambda k, pb, cs_, c0, N, wb_w: lambda e: e.matmul(psb[pb + 3][:, 0:N], lhsT=wb_w[:, k, cs_], rhs=obT[:, k, c0:c0 + N], start=(k == 0), stop=(k == 3)))(k, pb, cs_, c0, N, wb_w),
                         r=[T_wb[bi], T_obT], w=[PS[pb + 3]])
                S.op("act", (lambda pb, tb, N: lambda e: e.activation(out=sga[tb][:, 0:N], in_=psb[pb][:, 0:N], func=AF.Sigmoid))(pb, tb, N), r=[PS[pb]], w=[T_tb[tb]])
                S.op("act", (lambda pb, tb, N: lambda e: e.activation(out=sgb[tb][:, 0:N], in_=psb[pb + 1][:, 0:N], func=AF.Sigmoid))(pb, tb, N), r=[PS[pb + 1]], w=[T_tb[tb]])
                S.op("dve", (lambda pb, tb, N: lambda e: e.tensor_tensor(out=tm1[tb][:, 0:N], in0=sga[tb][:, 0:N], in1=psb[pb + 2][:, 0:N], op=ALU.mult))(pb, tb, N), r=[PS[pb + 2], T_tb[tb]], w=[T_tb[tb]])
                S.op("dve", (lambda pb, tb, N: lambda e: e.tensor_tensor(out=sgb[tb][:, 0:N], in0=sgb[tb][:, 0:N], in1=psb[pb + 3][:, 0:N], op=ALU.mult))(pb, tb, N), r=[PS[pb + 3], T_tb[tb]], w=[T_tb[tb]])
                S.op("dve", (lambda tb, N, c0, cidx: lambda e: e.tensor_tensor(out=mT[:, cidx, c0:c0 + N], in0=tm1[tb][:, 0:N], in1=sgb[tb][:, 0:N], op=ALU.add))(tb, N, c0, gi * 2 + cc),
                     r=[T_tb[tb]], w=[T_mT])

    S.barrier()
    acc = carve(0, [9, 2048], F32); T_acc = [S.T("acc%d" % i) for i in range(9)]
    O_WO = 73728
    wo_sb = [carve(O_WO + i * 16384, [16, 512], BF16) for i in range(2)]; T_wo = [S.T("wo0"), S.T("wo1")]
    O_XR = O_WO + 32768
    xr = [carve(O_XR + i * 2048, [512], F32) for i in range(2)]; T_xr = [S.T("xr0"), S.T("xr1")]
    w_out_v = w_out.rearrange("(k p) c -> p k c", p=128)
    it = 0
    for dg in range(4):
        bi = dg % 2
        S.dma("pool", wo_sb[bi][:, :, :], w_out_v[:, :, dg * 512:(dg + 1) * 512], w=[T_wo[bi]])
        for i in range(9):
            pb = it % 2; xb = it % 2; it += 1
            S.dma("sp", xr[xb][:, :], xo[i * 128:(i + 1) * 128, dg * 512:(dg + 1) * 512], w=[T_xr[xb]])
            for k in range(16):
                S.op("pe", (lambda k, pb, i, bi: lambda e: e.matmul(psb[pb][:, :], lhsT=mT[:, k, i * 128:(i + 1) * 128], rhs=wo_sb[bi][:, k, :], start=(k == 0), stop=(k == 15)))(k, pb, i, bi),
                     r=[T_mT, T_wo[bi]], w=[PS[pb]])
            S.op("dve", (lambda pb, xb, i, dg: lambda e: e.scalar_tensor_tensor(out=acc[:, i, dg * 512:(dg + 1) * 512], in0=xr[xb][:, :], scalar=ALPHA, in1=psb[pb][:, :],
                                                                               op0=ALU.mult, op1=ALU.add))(pb, xb, i, dg), r=[PS[pb], T_xr[xb]], w=[T_acc[i]])
    S.barrier()
    O_L = 110592
    lng = carve(O_L, [2048], F32); lnb = carve(O_L + 8192, [2048], F32); T_ln = S.T("lnp")
    hTs = carve(O_L + 16384, [16, 128], F32); T_hT = S.T("hT")
    O_S = O_L + 16384 + 8192
    o = O_S
    wrt = cv([16, 32], F32); brt = cv([32], F32); T_wrt = S.T("wrt")
    stats = cv([4, 6], F32); mv = cv([2], F32); rs1 = cv([2], F32); T_lns = S.T("ln_small")
    lg = cv([32], F32); top8 = cv([8], F32); negmx = cv([2], F32); ex_t = cv([32], F32); den = cv([2], F32); T_rt = S.T("rt_small")
    assert o <= 147456 - 3 * 1152 - 1152
    O_G = 73728
    gates = carve(O_G, [9, 32], F32); posm = carve(O_G + 1152, [9, 32], F32); mskf = carve(O_G + 2304, [9, 32], F32)
    mskb = carve(O_G + 3456, [9, 32], BF16)
    T_gate = S.T("gates")
    hbf = carve(O_MT, [9, 2048], BF16); T_hbf = S.T("hbf")

    def layer_norm(i, grow, brow):
        for c4 in range(4):
            S.op("dve", (lambda c4: lambda e: e.bn_stats(out=stats[:, c4, :], in_=acc[:, i, c4 * 512:(c4 + 1) * 512]))(c4), r=[T_acc[i]], w=[T_lns])
        S.op("dve", lambda e: e.bn_aggr(out=mv[:, 0:2], in_=stats[:, :, :]), r=[T_lns], w=[T_lns])
        S.op("dve", lambda e: e.tensor_scalar(out=rs1[:, 0:1], in0=mv[:, 1:2], scalar1=1e-5, scalar2=1.0, op0=ALU.add, op1=ALU.mult), r=[T_lns], w=[T_lns])
        S.op("act", lambda e: e.activation(out=rs1[:, 0:1], in_=rs1[:, 0:1], func=AF.Sqrt), r=[T_lns], w=[T_lns])
        S.op("dve", lambda e: e.reciprocal(out=rs1[:, 0:1], in_=rs1[:, 0:1]), r=[T_lns], w=[T_lns])
        S.op("dve", lambda e: e.tensor_scalar(out=acc[:, i, :], in0=acc[:, i, :], scalar1=mv[:, 0:1], scalar2=rs1[:, 0:1], op0=ALU.subtract, op1=ALU.mult), r=[T_lns, T_acc[i]], w=[T_acc[i]])
        S.op("dve", lambda e: e.tensor_tensor(out=acc[:, i, :], in0=acc[:, i, :], in1=lng[:, :], op=ALU.mult), r=[T_ln, T_acc[i]], w=[T_acc[i]])
        S.op("dve", lambda e: e.tensor_tensor(out=acc[:, i, :], in0=acc[:, i, :], in1=lnb[:, :], op=ALU.add), r=[T_ln, T_acc[i]], w=[T_acc[i]])

    S.dma("sp", lng[:, :], lnp[0:1, :].broadcast_to([128, 2048]), w=[T_ln])
    S.dma("sp", lnb[:, :], lnp[1:2, :].broadcast_to([128, 2048]), w=[T_ln])
    S.dma("sp", wrt[:, :, :], w_rt.rearrange("(k p) c -> p k c", p=128), w=[T_wrt])
    S.dma("sp", brt[:, :], b_rt[0:1, :].broadcast_to([128, 32]), w=[T_wrt])
    for i in range(9):
        layer_norm(i, 0, 1)
        if dbg:
            pass
        for q4 in range(4):
            pb = q4 % 2
            for kk in range(4):
                k = q4 * 4 + kk
                S.op("pe", (lambda pb, kk, k, i: lambda e: e.transpose(out=psb[pb][:, kk * 128:(kk + 1) * 128], in_=acc[:, i, k * 128:(k + 1) * 128], identity=idf[:, :]))(pb, kk, k, i),
                     r=[T_acc[i], T_c], w=[PS[pb]])
            evac(hTs[:, q4 * 4:(q4 + 1) * 4, :], psb[pb][:, :].rearrange("p (a b) -> p a b", b=128), [PS[pb]], [T_hT])
        for k in range(16):
            S.op("pe", (lambda k: lambda e: e.matmul(psb[2][:, 0:32], lhsT=hTs[:, k, :], rhs=wrt[:, k, :], start=(k == 0), stop=(k == 15)))(k), r=[T_hT, T_wrt], w=[PS[2]])
        S.op("dve", lambda e: e.tensor_tensor(out=lg[:, :], in0=psb[2][:, 0:32], in1=brt[:, :], op=ALU.add), r=[PS[2], T_wrt], w=[T_rt])
        S.op("dve", lambda e: e.max(out=top8[:, :], in_=lg[:, :]), r=[T_rt], w=[T_rt])
        S.op("dve", (lambda i: lambda e: e.tensor_scalar(out=mskf[:, i, :], in0=lg[:, :], scalar1=top8[:, 3:4], scalar2=tval[:, i:i + 1], op0=ALU.is_ge, op1=ALU.mult))(i),
             r=[T_rt, T_c], w=[T_gate])
        S.op("dve", lambda e: e.tensor_scalar(out=negmx[:, 0:1], in0=top8[:, 0:1], scalar1=-1.0, scalar2=0.0, op0=ALU.mult, op1=ALU.add), r=[T_rt], w=[T_rt])
        S.op("act", lambda e: e.activation(out=ex_t[:, :], in_=lg[:, :], func=AF.Exp, bias=negmx[:, 0:1], scale=1.0), r=[T_rt], w=[T_rt])
        S.op("dve", (lambda i: lambda e: e.scalar_tensor_tensor(out=ex_t[:, :], in0=ex_t[:, :], scalar=1.0, in1=mskf[:, i, :], op0=ALU.mult, op1=ALU.mult, accum_out=den[:, 0:1]))(i),
             r=[T_rt, T_gate], w=[T_rt])
        S.op("dve", lambda e: e.tensor_scalar(out=den[:, 0:1], in0=den[:, 0:1], scalar1=1e-30, scalar2=1.0, op0=ALU.add, op1=ALU.mult), r=[T_rt], w=[T_rt])
        S.op("dve", lambda e: e.reciprocal(out=den[:, 0:1], in_=den[:, 0:1]), r=[T_rt], w=[T_rt])
        S.op("dve", (lambda i: lambda e: e.tensor_scalar(out=gates[:, i, :], in0=ex_t[:, :], scalar1=den[:, 0:1], scalar2=1.0, op0=ALU.mult, op1=ALU.mult))(i), r=[T_rt], w=[T_gate])
        S.op("act", (lambda i: lambda e: e.copy(out=mskb[:, i, :], in_=mskf[:, i, :]))(i), r=[T_gate], w=[T_gate])
        for i2 in range(i + 1):
            S.op("pe", (lambda i2, i: lambda e: e.matmul(psb[3][:, 0:32], lhsT=(onesb[:, :] if i2 < i else ustr[:, :]), rhs=mskb[:, i2, :], start=(i2 == 0), stop=(i2 == i)))(i2, i),
                 r=[T_gate, T_c], w=[PS[3]])
        S.op("dve", (lambda i: lambda e: e.scalar_tensor_tensor(out=posm[:, i, :], in0=psb[3][:, 0:32], scalar=1.0, in1=mskf[:, i, :], op0=ALU.add, op1=ALU.mult))(i),
             r=[PS[3], T_gate], w=[T_gate])
        S.op("dve", (lambda i: lambda e: e.tensor_scalar(out=posm[:, i, :], in0=posm[:, i, :], scalar1=-1.0, scalar2=1.0, op0=ALU.add, op1=ALU.mult))(i), r=[T_gate], w=[T_gate])
        S.op("act", (lambda i: lambda e: e.copy(out=hbf[:, i, :], in_=acc[:, i, :]))(i), r=[T_acc[i]], w=[T_hbf])
        if dbg and i == 8:
            d_h = dout("d_h", [TOK, D])
            for i3 in range(9):
                outs_final.append(S.dma("sp", d_h[i3 * 128:(i3 + 1) * 128, :], acc[:, i3, :], r=[T_acc[i3]]))
    if dbg:
        d_gates = dout("d_gates", [128, 9 * 32]); d_posm = dout("d_posm", [128, 9 * 32])
        outs_final.append(S.dma("sp", d_gates, gates.rearrange("p a b -> p (a b)"), r=[T_gate]))
        outs_final.append(S.dma("sp", d_posm, posm.rearrange("p a b -> p (a b)"), r=[T_gate]))
    for i in range(9):
        S.op("act", (lambda i: lambda e: e.mul(out=acc[:, i, :], in_=acc[:, i, :], mul=ALPHA))(i), r=[T_acc[i]], w=[T_acc[i]])

    S.barrier()
    O_C = O_G + 4608
    o = O_C
    wr = [cv([16, 256], BF16) for _ in range(3)]; T_wr = [S.T("wr%d" % i) for i in range(3)]
    Sel = cv([9, CAP], BF16); T_Sel = S.T("Sel")
    SelG = cv([9, CAP], BF16); T_SelG = S.T("SelG")
    SelGT = cv([2, 9, 128], BF16); T_SelGT = S.T("SelGT")
    xe = cv([16, CAP], BF16); T_xe = S.T("xe")
    actT = cv([16, CAP], BF16); T_actT = S.T("actT")
    ysb = cv([2, 2048], BF16); T_ysb = S.T("ysb")
    mg = [cv([CAP], F32) for _ in range(2)]; msg = [cv([CAP], F32) for _ in range(2)]; T_mo = [S.T("mo0"), S.T("mo1")]
    assert o <= O_MT, o
    wri = [0]
    def wload(src_ap):
        b = wri[0] % 3; wri[0] += 1
        S.dma("pool", wr[b][:, :, :], src_ap, w=[T_wr[b]])
        return b
    for ex in range(NEXP):
        for i in range(9):
            S.op("dve", (lambda i, ex: lambda e: e.tensor_scalar(out=Sel[:, i, :], in0=iota[:, :], scalar1=posm[:, i, ex:ex + 1], scalar2=1.0, op0=ALU.is_equal, op1=ALU.mult))(i, ex),
                 r=[T_gate, T_c], w=[T_Sel])
            S.op("dve", (lambda i, ex: lambda e: e.tensor_scalar(out=SelG[:, i, :], in0=iota[:, :], scalar1=posm[:, i, ex:ex + 1], scalar2=gates[:, i, ex:ex + 1], op0=ALU.is_equal, op1=ALU.mult))(i, ex),
                 r=[T_gate, T_c], w=[T_SelG])
        for k2 in range(8):
            pb = k2 % 2
            for kk in range(2):
                k = k2 * 2 + kk
                for i in range(9):
                    S.op("pe", (lambda pb, kk, k, i: lambda e: e.matmul(psb[pb][:, kk * CAP:(kk + 1) * CAP], lhsT=hbf[:, i, k * 128:(k + 1) * 128], rhs=Sel[:, i, :], start=(i == 0), stop=(i == 8)))(pb, kk, k, i),
                         r=[T_hbf, T_Sel], w=[PS[pb]])
            evac(xe[:, k2 * 2:(k2 + 1) * 2, :], psb[pb][:, :].rearrange("p (a b) -> p a b", b=CAP), [PS[pb]], [T_xe])
        for sc in range(2):
            for half in range(2):
                pb = 2 + half
                Pv = psb[pb][:].bitcast(BF16)
                ilist = range(0, 8) if half == 0 else range(8, 9)
                for ii, i in enumerate(ilist):
                    S.op("pe", (lambda Pv, ii, i, sc: lambda e: e.transpose(out=Pv[:, ii * 128:(ii + 1) * 128], in_=SelG[:, i, sc * 128:(sc + 1) * 128], identity=idb[:, :]))(Pv, ii, i, sc),
                         r=[T_SelG, T_c], w=[PS[pb]])
                n_i = len(ilist)
                evac(SelGT[:, sc, ilist[0]:ilist[0] + n_i, :], Pv[:, 0:n_i * 128].rearrange("p (a b) -> p a b", b=128), [PS[pb]], [T_SelGT])
        for c in range(16):
            wbuf = wload(w_gu[ex, c].rearrange("(k p) c -> p k c", p=128))
            pb = 4 + (c % 2); tb = c % 2
            for half in range(2):
                for k in range(16):
                    S.op("pe", (lambda pb, half, k, wbuf: lambda e: e.matmul(psb[pb][:, half * CAP:(half + 1) * CAP], lhsT=wr[wbuf][:, k, half * 128:(half + 1) * 128], rhs=xe[:, k, :],
                                                                          start=(k == 0), stop=(k == 15)))(pb, half, k, wbuf), r=[T_wr[wbuf], T_xe], w=[PS[pb]])
            bcol = ex * 32 + c * 2
            S.op("dve", (lambda pb, tb, bcol: lambda e: e.tensor_scalar(out=mg[tb][:, :], in0=psb[pb][:, 0:CAP], scalar1=bgu[:, bcol:bcol + 1], scalar2=7.0, op0=ALU.add, op1=ALU.min))(pb, tb, bcol),
                 r=[PS[pb], T_c], w=[T_mo[tb]])
            S.op("act", (lambda tb: lambda e: e.activation(out=msg[tb][:, :], in_=mg[tb][:, :], func=AF.Sigmoid, scale=1.702))(tb), r=[T_mo[tb]], w=[T_mo[tb]])
            S.op("dve", (lambda tb: lambda e: e.tensor_tensor(out=mg[tb][:, :], in0=mg[tb][:, :], in1=msg[tb][:, :], op=ALU.mult))(tb), r=[T_mo[tb]], w=[T_mo[tb]])
            S.op("dve", (lambda pb, tb, bcol: lambda e: e.tensor_scalar(out=msg[tb][:, :], in0=psb[pb][:, CAP:2 * CAP], scalar1=bgu[:, bcol + 1:bcol + 2], scalar2=7.0, op0=ALU.add, op1=ALU.min))(pb, tb, bcol),
                 r=[PS[pb], T_c, T_mo[tb]], w=[T_mo[tb]])
            S.op("dve", (lambda tb: lambda e: e.tensor_scalar(out=msg[tb][:, :], in0=msg[tb][:, :], scalar1=-7.0, scalar2=1.0, op0=ALU.max, op1=ALU.add))(tb), r=[T_mo[tb]], w=[T_mo[tb]])
            S.op("dve", (lambda tb, c: lambda e: e.tensor_tensor(out=actT[:, c, :], in0=mg[tb][:, :], in1=msg[tb][:, :], op=ALU.mult))(tb, c), r=[T_mo[tb]], w=[T_actT])
        for j in range(8):
            wbuf = wload(w_dn[ex].rearrange("(k p) c -> p k c", p=128)[:, :, j * 256:(j + 1) * 256])
            pb = 6 + (j % 2)
            for sc in range(2):
                for k in range(16):
                    S.op("pe", (lambda pb, sc, k, wbuf: lambda e: e.matmul(psb[pb][:, sc * 256:(sc + 1) * 256], lhsT=actT[:, k, sc * 128:(sc + 1) * 128], rhs=wr[wbuf][:, k, :],
                                                                        start=(k == 0), stop=(k == 15)))(pb, sc, k, wbuf), r=[T_wr[wbuf], T_actT], w=[PS[pb]])
            evac(ysb[:, :, j * 256:(j + 1) * 256], psb[pb][:, :].rearrange("p (a b) -> p a b", b=256), [PS[pb]], [T_ysb])
        it = 0
        for i in range(9):
            for dg in range(4):
                pb = it % 2; it += 1
                for sc in range(2):
                    S.op("pe", (lambda pb, sc, i, dg: lambda e: e.matmul(psb[pb][:, :], lhsT=SelGT[:, sc, i, :], rhs=ysb[:, sc, dg * 512:(dg + 1) * 512], start=(sc == 0), stop=(sc == 1)))(pb, sc, i, dg),
                         r=[T_SelGT, T_ysb], w=[PS[pb]])
                S.op("dve", (lambda pb, i, dg: lambda e: e.tensor_tensor(out=acc[:, i, dg * 512:(dg + 1) * 512], in0=acc[:, i, dg * 512:(dg + 1) * 512], in1=psb[pb][:, :], op=ALU.add))(pb, i, dg),
                     r=[PS[pb], T_acc[i]], w=[T_acc[i]])

    S.barrier()
    o = O_C
    gT = cv([9, 128], F32); bdn = cv([2048], F32); T_bd = S.T("bd")
    S.op("dve", lambda e: e.memset(gT[:, :, :], 0.0), w=[T_bd])
    S.op("dve", lambda e: e.memset(bdn[:, :], 0.0), w=[T_bd])
    S.dma("sp", bdn[0:32, :], b_dn, w=[T_bd])
    for i in range(9):
        S.op("pe", (lambda i: lambda e: e.transpose(out=psb[2][0:32, 0:128], in_=gates[:, i, :], identity=idf[:, :]))(i), r=[T_gate, T_c], w=[PS[2]])
        S.op("act", (lambda i: lambda e: e.mul(out=gT[0:32, i, :], in_=psb[2][0:32, 0:128], mul=1.0))(i), r=[PS[2]], w=[T_bd])
    it = 0
    for i in range(9):
        for dg in range(4):
            pb = it % 2; it += 1
            S.op("pe", (lambda pb, i, dg: lambda e: e.matmul(psb[pb][:, :], lhsT=gT[:, i, :], rhs=bdn[:, dg * 512:(dg + 1) * 512], start=True, stop=True))(pb, i, dg),
                 r=[T_bd], w=[PS[pb]])
            S.op("dve", (lambda pb, i, dg: lambda e: e.tensor_tensor(out=acc[:, i, dg * 512:(dg + 1) * 512], in0=acc[:, i, dg * 512:(dg + 1) * 512], in1=psb[pb][:, :], op=ALU.add))(pb, i, dg),
                 r=[PS[pb], T_acc[i]], w=[T_acc[i]])

    S.dma("sp", lng[:, :], lnp[2:3, :].broadcast_to([128, 2048]), w=[T_ln])
    S.dma("sp", lnb[:, :], lnp[3:4, :].broadcast_to([128, 2048]), w=[T_ln])
    for i in range(9):
        layer_norm(i, 2, 3)
        outs_final.append(S.dma("sp", y_d[i * 128:(i + 1) * 128, :], acc[:, i, :], r=[T_acc[i]]))

    dbg_outs = {}
    if dbg:
        pass


    S.barrier()
    S.emit(outs_final)
    print("[kernel] sched stats", S.stats, flush=True)
    return nc, stack


def _consts():
    import ml_dtypes
    c = {}
    c["c_idb"] = np.eye(128, dtype=np.float32)
    c["c_idf"] = np.eye(128, dtype=np.float32)
    m = np.zeros((128, 64), np.float32)
    for s in range(64):
        m[s, s:] = 1.0
    c["c_m64"] = m
    dm = np.full((128, 3072), BIG, np.float32)
    for g in range(3):
        W, dl = WINS[g], DILS[g]
        q = np.arange(128)[:, None]
        k = np.arange(W + 128)[None, :]
        dist = (W + q) - k
        ok = (dist >= 0) & (dist <= W) & (dist % dl == 0)
        dm[:, GOFF[g]:GOFF[g] + W + 128] = np.where(ok, dist, BIG)
    c["c_dm"] = dm
    c["c_iota"] = np.tile(np.arange(CAP, dtype=np.float32)[None, :], (128, 1))
    u = np.zeros((128, 128), np.float32)
    for a in range(128):
        u[a, a + 1:] = 1.0
    c["c_ustr"] = u
    tv = np.ones((128, 9), np.float32)
    tv[32:, 8] = 0.0
    c["c_tval"] = tv
    return c


def _prep(inp):
    f = np.float32
    w_in = inp["w_in"][0]
    SP = np.cumsum([0, 1024, 1024, 1024, 1024, 1536, 512, 512, 2048, 2048])
    sec = {n: w_in[:, SP[i]:SP[i + 1]] for i, n in enumerate(["hq", "hf", "hi", "hg", "aq", "ak", "av", "ga", "gb"])}
    w_hg = np.stack([np.concatenate([sec[n][:, h * 128:(h + 1) * 128] for n in ("hq", "hf", "hi", "hg")], axis=1) for h in range(8)])
    w_at = np.stack([np.concatenate([sec["aq"][:, (g * 4 + h) * 128:(g * 4 + h + 1) * 128] for g in range(3)]
                                    + [sec["ak"][:, h * 128:(h + 1) * 128], sec["av"][:, h * 128:(h + 1) * 128]], axis=1) for h in range(4)])
    w_gate = np.ascontiguousarray(np.concatenate([sec["ga"], sec["gb"]], axis=1))
    wgu = inp["w_gate_up"][0]
    wgu_r = wgu.reshape(NEXP, D, 16, 128, 2)
    w_gu = np.ascontiguousarray(wgu_r.transpose(0, 2, 1, 4, 3).reshape(NEXP, 16, D, 256))
    bgu = inp["b_gate_up"][0].reshape(NEXP, 16, 128, 2)
    b_gu = np.ascontiguousarray(bgu.transpose(2, 0, 1, 3).reshape(128, NEXP * 32))
    lnp = np.stack([inp["ln1_g"][0], inp["ln1_b"][0], inp["ln2_g"][0], inp["ln2_b"][0]]).astype(f)
    lb = inp["hgrn_lb_logits"]
    lbl = np.ascontiguousarray(np.concatenate([lb[0].reshape(8, 128).T, lb[1].reshape(8, 128).T], axis=1))
    shared = dict(w_hg=np.ascontiguousarray(w_hg), w_at=np.ascontiguousarray(w_at), w_gate=w_gate,
                  w_ba=inp["w_branch_a"][0], w_bb=inp["w_branch_b"][0], w_out=inp["w_out"][0],
                  w_rt=inp["w_router"][0], b_rt=inp["b_router"][0].reshape(1, 32), w_gu=w_gu, b_gu=b_gu,
                  w_dn=inp["w_down"][0], b_dn=inp["b_down"][0], lnp=lnp, lbl=lbl,
                  nrmw=np.ascontiguousarray(inp["hgrn_norm_w"][0].reshape(128, 1)))
    shared.update(_consts())
    maps = []
    xp_all = inp["x_prompt"]
    for c in range(NCORES):
        seq, seg = c // 4, c % 4
        xown = np.zeros((TOK, D), f)
        xown[0:1024] = xp_all[seq, seg * 1024:(seg + 1) * 1024]
        xown[1024:1056] = inp["x_sample"][4 * c:4 * c + 4].reshape(32, D)
        pre = np.zeros((NPREF, D), f)
        npre = seg * 1024
        if npre:
            pre[NPREF - npre:] = xp_all[seq, 0:npre]
        kb = np.zeros((1, 2048), f)
        nval = min(npre, 2048)
        kb[0, :2048 - nval] = -1.0e6
        m = dict(shared)
        m.update(xoT=np.ascontiguousarray(xown.T), xo=xown, xpT=np.ascontiguousarray(pre.T), kbias=kb,
                 kc=np.ascontiguousarray(inp["cache_attn_k"][0, 4 * c:4 * c + 4].reshape(4, 2048, 512)),
                 vc=np.ascontiguousarray(inp["cache_attn_v"][0, 4 * c:4 * c + 4].reshape(4, 2048, 512)),
                 st0=np.ascontiguousarray(inp["state_hgrn"][0, 4 * c:4 * c + 4]))
        maps.append(m)
    return maps


def _assemble(res):
    f = np.float32
    y_p = np.zeros((2, 4096, D), f); y_s = np.zeros((32, 8, D), f)
    kp = np.zeros((1, 2, 2048, 4, 128), f); vp = np.zeros((1, 2, 2048, 4, 128), f)
    sp = np.zeros((1, 2, 8, 128, 128), f)
    ks = np.zeros((1, 32, 8, 4, 128), f); vs = np.zeros((1, 32, 8, 4, 128), f)
    ss = np.zeros((1, 32, 8, 128, 128), f)
    for c in range(NCORES):
        r = res[c]
        seq, seg = c // 4, c % 4
        y = r["y"]
        y_p[seq, seg * 1024:(seg + 1) * 1024] = y[0:1024]
        y_s[4 * c:4 * c + 4] = y[1024:1056].reshape(4, 8, D)
        kn = r["knT"].T
        vn = r["vn"]
        if seg >= 2:
            kp[0, seq, (seg - 2) * 1024:(seg - 1) * 1024] = kn[0:1024].reshape(1024, 4, 128)
            vp[0, seq, (seg - 2) * 1024:(seg - 1) * 1024] = vn[0:1024].reshape(1024, 4, 128)
        ks[0, 4 * c:4 * c + 4] = kn[1024:1056].reshape(4, 8, 4, 128)
        vs[0, 4 * c:4 * c + 4] = vn[1024:1056].reshape(4, 8, 4, 128)
        if seg == 3:
            sp[0, seq] = r["stp"]
        ss[0, 4 * c:4 * c + 4] = r["sts"]
    return (y_p, y_s, kp, vp, sp, ks, vs, ss)


def kernel(**inputs):
    inp = {k: np.asarray(v) for k, v in inputs.items()}
    maps = _prep(inp)
    nc, stack = build_nc()
    with stack:
        pass
    res = run_bass_kernel_spmd(nc, maps, core_ids=list(range(NCORES)))
    return _assemble(res.results)
```
